# Optimizing a Trainium2 kernel written in Bass

```python
import jax, jax.numpy as jnp
from jax import lax
import numpy as np

D_MODEL = 1024
BATCH = 8
SEQ = 4096
DEPTH = 2

CTX_LEN = 256
GRID_W = 64

HEAD_DIM = 128
N_Q_HEADS = 4
N_KV_HEADS = 2
REP = N_Q_HEADS // N_KV_HEADS
ATTN_W = N_Q_HEADS * HEAD_DIM
KV_W = N_KV_HEADS * HEAD_DIM
AXIS_DIM = HEAD_DIM // 2
ROPE_THETA = 10000.0
Q_BLOCK = 128
SCONV_W = 256
SCONV_K = 3
FOURIER_W = 256
FOURIER_GROUPS = 4
FOURIER_GROUP_W = FOURIER_W // FOURIER_GROUPS
CONF_W = 256
CONF_K = 31
MIX_W = ATTN_W + SCONV_W + FOURIER_W + CONF_W
Q_END = ATTN_W
K_END = Q_END + KV_W
V_END = K_END + KV_W
IN_W = V_END + 3 * SCONV_W + FOURIER_W + 2 * CONF_W
N_EXPERTS = 32
TOP_K = 4
D_FF = D_MODEL
SWIGLU_LIMIT = 7.0
SWIGLU_ALPHA = 1.702
N_MOD = 6
EPS = 1e-6

kernel_name = 'hybrid_parallel_groups_flow_backbone'


def rms_norm(x, g):
    xf = x.astype(jnp.float32)
    y = xf * lax.rsqrt(jnp.mean(xf * xf, axis=-1, keepdims=True) + EPS)
    return (y * g.astype(jnp.float32)).astype(x.dtype)


def layer_norm(x, g, b):
    xf = x.astype(jnp.float32)
    mu = jnp.mean(xf, axis=-1, keepdims=True)
    var = jnp.mean(jnp.square(xf - mu), axis=-1, keepdims=True)
    y = (xf - mu) * lax.rsqrt(var + EPS)
    return (y * g.astype(jnp.float32) + b.astype(jnp.float32)).astype(x.dtype)


def modulate(h, shift, scale):
    return h * (1 + scale) + shift


def axial_rope_tables(rows, dtype):
    row = jnp.broadcast_to(jnp.arange(rows, dtype=jnp.float32)[:, None], (rows, GRID_W)).reshape(-1)
    col = jnp.broadcast_to(jnp.arange(GRID_W, dtype=jnp.float32)[None, :], (rows, GRID_W)).reshape(-1)
    inv_freq = ROPE_THETA ** (-jnp.arange(AXIS_DIM // 2, dtype=jnp.float32) * 2.0 / AXIS_DIM)
    ar = row[:, None] * inv_freq
    ac = col[:, None] * inv_freq
    ang = jnp.concatenate([ar, ar, ac, ac], axis=-1)
    return jnp.cos(ang).astype(dtype), jnp.sin(ang).astype(dtype)


def rotate_half_axial(x):
    xr = x.reshape(x.shape[:-1] + (2, 2, AXIS_DIM // 2))
    x1 = xr[..., 0, :]
    x2 = xr[..., 1, :]
    return jnp.stack([-x2, x1], axis=-2).reshape(x.shape)


def apply_rope(x, cos, sin):
    return x * cos[None, :, None, :] + rotate_half_axial(x) * sin[None, :, None, :]


def split_heads(t, n_heads):
    return t.reshape(t.shape[:-1] + (n_heads, HEAD_DIM))


def attend(qg, k, v):
    s = jnp.einsum('bqgrd,bkgd->bgrqk', qg, k, preferred_element_type=jnp.float32) * (HEAD_DIM ** -0.5)
    p = jax.nn.softmax(s, axis=-1)
    return jnp.einsum('bgrqk,bkgd->bqgrd', p.astype(v.dtype), v)


def block_attention(q, k, v):
    b_, s_ = q.shape[0], q.shape[1]
    n_blk = s_ // Q_BLOCK
    qb = jnp.moveaxis(q.reshape(b_, n_blk, Q_BLOCK, N_KV_HEADS, REP, HEAD_DIM), 1, 0)
    o = lax.map(lambda qblk: attend(qblk, k, v), qb)
    return jnp.moveaxis(o, 0, 1).reshape(b_, s_, ATTN_W)


def depthwise_conv(x, w):
    k_w, ch = w.shape
    return lax.conv_general_dilated(
        x, w[:, None, :], window_strides=(1,), padding=[(k_w // 2, k_w // 2)],
        dimension_numbers=('NWC', 'WIO', 'NWC'), feature_group_count=ch)


def fourier_mix(f):
    b_, s_, _ = f.shape
    fg = f.reshape(b_, s_, FOURIER_GROUPS, FOURIER_GROUP_W).astype(jnp.float32)
    y = jnp.fft.fft2(fg, axes=(1, 3), norm='ortho').real
    return y.reshape(b_, s_, FOURIER_W).astype(f.dtype)


def local_mixers(u_loc, sconv_w, conf_dw_w, conf_dw_b, conf_ln_g, conf_ln_b):
    sb, sc, sx, fo, cf = jnp.split(
        u_loc, [SCONV_W, 2 * SCONV_W, 3 * SCONV_W, 3 * SCONV_W + FOURIER_W], axis=-1)
    y_sconv = sb * depthwise_conv(sc * sx, sconv_w)
    y_four = fourier_mix(fo)
    a, g = jnp.split(cf, 2, axis=-1)
    d = depthwise_conv(a * jax.nn.sigmoid(g), conf_dw_w) + conf_dw_b
    y_conf = jax.nn.silu(layer_norm(d, conf_ln_g, conf_ln_b))
    return jnp.concatenate([y_sconv, y_four, y_conf], axis=-1)


def moe(h, router_w, router_b, w_up, b_up, w_down, b_down):
    shape = h.shape
    ht = h.reshape(-1, D_MODEL)
    logits = (ht @ router_w).astype(jnp.float32) + router_b.astype(jnp.float32)
    top_vals, top_idx = lax.top_k(logits, TOP_K)
    probs = jax.nn.softmax(top_vals, axis=-1)
    combine = jnp.sum(jax.nn.one_hot(top_idx, N_EXPERTS, dtype=jnp.float32) * probs[..., None], axis=1)
    out = jnp.zeros((ht.shape[0], D_MODEL), jnp.float32)
    for e in range(N_EXPERTS):
        gu = ht @ w_up[e] + b_up[e]
        gate = jnp.minimum(gu[:, ::2], SWIGLU_LIMIT)
        lin = jnp.clip(gu[:, 1::2], -SWIGLU_LIMIT, SWIGLU_LIMIT)
        act = gate * jax.nn.sigmoid(SWIGLU_ALPHA * gate) * (lin + 1)
        y = act @ w_down[e] + b_down[e]
        out = out + combine[:, e:e + 1] * y.astype(jnp.float32)
    return out.astype(h.dtype).reshape(shape)


def hybrid_layer(x, xc, c_act, cc_act, cos, sin, w_mod, b_mod, norm_mix, norm_ffn, w_in,
                 q_norm, k_norm, sconv_w, conf_dw_w, conf_dw_b, conf_ln_g, conf_ln_b, w_out,
                 router_w, router_b, w_up, b_up, w_down, b_down, update_ctx):
    b_, s_ = x.shape[0], x.shape[1]
    s_c = xc.shape[1]
    sh1, sc1, g1, sh2, sc2, g2 = jnp.split((c_act @ w_mod + b_mod)[:, None, :], N_MOD, axis=-1)
    csh1, csc1, cg1, csh2, csc2, cg2 = jnp.split((cc_act @ w_mod + b_mod)[None, None, :], N_MOD, axis=-1)

    h = modulate(rms_norm(x, norm_mix), sh1, sc1)
    hc = modulate(rms_norm(xc, norm_mix), csh1, csc1)
    u = h @ w_in
    if update_ctx:
        uc = hc @ w_in
        uc_kv = uc[..., Q_END:V_END]
    else:
        uc_kv = hc @ w_in[:, Q_END:V_END]
    kc = rms_norm(split_heads(uc_kv[..., :KV_W], N_KV_HEADS), k_norm)
    vc = split_heads(uc_kv[..., KV_W:], N_KV_HEADS)
    q = apply_rope(rms_norm(split_heads(u[..., :Q_END], N_Q_HEADS), q_norm), cos, sin)
    k = apply_rope(rms_norm(split_heads(u[..., Q_END:K_END], N_KV_HEADS), k_norm), cos, sin)
    v = split_heads(u[..., K_END:V_END], N_KV_HEADS)
    k_all = jnp.concatenate([kc, k], axis=1)
    v_all = jnp.concatenate([vc, v], axis=1)
    y_attn = block_attention(q, k_all, v_all)
    y = jnp.concatenate(
        [y_attn, local_mixers(u[..., V_END:], sconv_w, conf_dw_w, conf_dw_b, conf_ln_g, conf_ln_b)],
        axis=-1) @ w_out
    x = x + g1 * y
    h2 = modulate(rms_norm(x, norm_ffn), sh2, sc2)

    if update_ctx:
        qc = rms_norm(split_heads(uc[..., :Q_END], N_Q_HEADS), q_norm)
        yc_attn = attend(qc.reshape(b_, s_c, N_KV_HEADS, REP, HEAD_DIM), kc, vc).reshape(b_, s_c, ATTN_W)
        yc = jnp.concatenate(
            [yc_attn, local_mixers(uc[..., V_END:], sconv_w, conf_dw_w, conf_dw_b, conf_ln_g, conf_ln_b)],
            axis=-1) @ w_out
        xc = xc + cg1 * yc
        h2c = modulate(rms_norm(xc, norm_ffn), csh2, csc2)
        ff = moe(jnp.concatenate([h2, h2c], axis=1), router_w, router_b, w_up, b_up, w_down, b_down)
        x = x + g2 * ff[:, :s_]
        xc = xc + cg2 * ff[:, s_:]
    else:
        x = x + g2 * moe(h2, router_w, router_b, w_up, b_up, w_down, b_down)
    return x, xc


def setup_inputs(seed: int = 0) -> dict:
    key = jax.random.key(seed)
    ks = jax.random.split(key, 24)
    L = DEPTH

    def nrm(k, shape, s):
        return s * jax.random.normal(k, shape, jnp.float32)

    return {
        'x': nrm(ks[0], (BATCH, SEQ, D_MODEL), 1.0),
        'c': nrm(ks[1], (BATCH, D_MODEL), 1.0),
        'ctx': nrm(ks[2], (BATCH, CTX_LEN, D_MODEL), 1.0),
        'c_ctx': nrm(ks[3], (D_MODEL,), 1.0),
        'w_mod': nrm(ks[4], (L, D_MODEL, N_MOD * D_MODEL), 0.5 * D_MODEL ** -0.5),
        'b_mod': nrm(ks[5], (L, N_MOD * D_MODEL), 0.01),
        'norm_mix': 1.0 + nrm(ks[6], (L, D_MODEL), 0.05),
        'norm_ffn': 1.0 + nrm(ks[7], (L, D_MODEL), 0.05),
        'w_in': nrm(ks[8], (L, D_MODEL, IN_W), D_MODEL ** -0.5),
        'q_norm': 1.0 + nrm(ks[9], (L, HEAD_DIM), 0.05),
        'k_norm': 1.0 + nrm(ks[10], (L, HEAD_DIM), 0.05),
        'sconv_w': nrm(ks[11], (L, SCONV_K, SCONV_W), SCONV_K ** -0.5),
        'conf_dw_w': nrm(ks[12], (L, CONF_K, CONF_W), CONF_K ** -0.5),
        'conf_dw_b': nrm(ks[13], (L, CONF_W), 0.01),
        'conf_ln_g': 1.0 + nrm(ks[14], (L, CONF_W), 0.05),
        'conf_ln_b': nrm(ks[15], (L, CONF_W), 0.01),
        'w_out': nrm(ks[16], (L, MIX_W, D_MODEL), MIX_W ** -0.5),
        'router_w': nrm(ks[17], (L, D_MODEL, N_EXPERTS), D_MODEL ** -0.5),
        'router_b': nrm(ks[18], (L, N_EXPERTS), 0.01),
        'w_up': nrm(ks[19], (L, N_EXPERTS, D_MODEL, 2 * D_FF), D_MODEL ** -0.5),
        'b_up': nrm(ks[20], (L, N_EXPERTS, 2 * D_FF), 0.01),
        'w_down': nrm(ks[21], (L, N_EXPERTS, D_FF, D_MODEL), D_FF ** -0.5),
        'b_down': nrm(ks[22], (L, N_EXPERTS, D_MODEL), 0.01),
        'final_norm': 1.0 + nrm(ks[23], (D_MODEL,), 0.05),
    }


def reference(x, c, ctx, c_ctx, w_mod, b_mod, norm_mix, norm_ffn, w_in, q_norm, k_norm,
              sconv_w, conf_dw_w, conf_dw_b, conf_ln_g, conf_ln_b, w_out, router_w, router_b,
              w_up, b_up, w_down, b_down, final_norm):
    rows = x.shape[1] // GRID_W
    cos, sin = axial_rope_tables(rows, x.dtype)
    c_act = jax.nn.silu(c)
    cc_act = jax.nn.silu(c_ctx)
    xc = ctx
    for l in range(DEPTH):
        x, xc = hybrid_layer(
            x, xc, c_act, cc_act, cos, sin, w_mod[l], b_mod[l], norm_mix[l], norm_ffn[l], w_in[l],
            q_norm[l], k_norm[l], sconv_w[l], conf_dw_w[l], conf_dw_b[l], conf_ln_g[l], conf_ln_b[l],
            w_out[l], router_w[l], router_b[l], w_up[l], b_up[l], w_down[l], b_down[l],
            update_ctx=(l < DEPTH - 1))
    return rms_norm(x, final_norm)
```

```python
import math
from contextlib import ExitStack
import numpy as np
import ml_dtypes
import concourse.bass as bass
import concourse.mybir as mybir
from concourse.bass_utils import run_bass_kernel_spmd

F32 = mybir.dt.float32
I32 = mybir.dt.int32
BF16 = mybir.dt.bfloat16
AF = mybir.ActivationFunctionType
ALU = mybir.AluOpType
AX = mybir.AxisListType

D = 1024
TS = 256
NTM = 100
S = 4096
CTX = 256
L = 2
E = 32
NT_LAT = 32
NT = 34
IN_W = 2560
EPS = 1e-6
NDMA = 24


class Buf:
    __slots__ = ("w", "r", "name", "psum")

    def __init__(self, name="", psum=False):
        self.w = []
        self.r = {}
        self.name = name
        self.psum = psum


class Eng:
    def __init__(self, name, e, sem):
        self.name = name
        self.e = e
        self.sem = sem
        self.cnt = 0
        self.seen = {}


class Slot:
    def __init__(self, key, sem):
        self.key = key
        self.sem = sem
        self.cnt = 0


class Trk:
    def __init__(self, nc, es):
        self.nc = nc
        self.eng = {}
        for name, e in (("pe", nc.tensor), ("act", nc.scalar), ("dve", nc.vector), ("pool", nc.gpsimd), ("sp", nc.sync)):
            self.eng[name] = Eng(name, e, es.enter_context(nc.semaphore("s_" + name)))
        self.slots = {}
        self.qi = {}
        for q in ("sp", "pool"):
            self.slots[q] = [Slot("d_%s%d" % (q, i), es.enter_context(nc.semaphore("d_%s%d" % (q, i)))) for i in range(NDMA)]
            self.qi[q] = 0
        self.pe_pending = set()

    def _deps(self, eng, reads, writes, is_dma=False):
        need = {}

        def add(tok):
            if tok is None or (tok[2] == eng.name and eng.name == "pe"):
                return
            if need.get(tok[0], (0, None))[0] < tok[1]:
                need[tok[0]] = (tok[1], tok[3])

        for b in reads:
            for t in b.w:
                add(t)
        for b in writes:
            for t in b.w:
                if not (is_dma and t[2] is None):
                    add(t)
            for t in b.r.values():
                add(t)
        for key, (cnt, sem) in need.items():
            if eng.seen.get(key, 0) >= cnt:
                continue
            eng.e.wait_ge(sem, cnt)
            eng.seen[key] = cnt

    def op(self, en, fn, reads=(), writes=()):
        eng = self.eng[en]
        writes = list(writes) + [b for b in reads if b.psum]
        reads = [b for b in reads if not b.psum]
        self._deps(eng, reads, writes)
        ins = fn(eng.e)
        eng.cnt += 1
        ins.then_inc(eng.sem, 1)
        tok = (en, eng.cnt, en, eng.sem)
        for b in reads:
            b.r[en] = tok
        for b in writes:
            b.w = [tok]
            b.r = {}
        return ins

    def mm(self, out_ap, lhsT, rhs, start, stop, reads, out_buf, inc=False, transpose=False):
        eng = self.eng["pe"]
        self._deps(eng, reads, [out_buf] if start else [])
        if transpose:
            ins = self.nc.tensor.transpose(out_ap, lhsT, rhs)
        else:
            ins = self.nc.tensor.matmul(out_ap, lhsT, rhs, start=start, stop=stop)
        self.pe_pending.update(reads)
        if stop or inc:
            eng.cnt += 1
            ins.then_inc(eng.sem, 1)
            tok = ("pe", eng.cnt, "pe", eng.sem)
            for b in self.pe_pending:
                b.r["pe"] = tok
            self.pe_pending = set()
            if stop:
                out_buf.w = [tok]
                out_buf.r = {}
        return ins

    def dma(self, q, out_ap, in_ap, reads=(), writes=(), **kw):
        eng = self.eng[q]
        sl = self.slots[q]
        slot = sl[self.qi[q] % len(sl)]
        self.qi[q] += 1
        if slot.cnt > 0 and eng.seen.get(slot.key, 0) < slot.cnt:
            eng.e.wait_ge(slot.sem, slot.cnt)
            eng.seen[slot.key] = slot.cnt
        self._deps(eng, reads, writes, is_dma=True)
        ins = eng.e.dma_start(out=out_ap, in_=in_ap, **kw)
        slot.cnt += 16
        ins.then_inc(slot.sem, 16)
        tok = (slot.key, slot.cnt, None, slot.sem)
        for b in reads:
            b.r[slot.key] = tok
        for b in writes:
            if b.w and all(t[2] is None for t in b.w) and not b.r:
                b.w = [t for t in b.w if t[0] != slot.key] + [tok]
            else:
                b.w = [tok]
            b.r = {}
        return ins

    def idma(self, kind, out_ap, idx_ap, in_ap, reads=(), writes=()):
        eng = self.eng["pool"]
        sl = self.slots["pool"]
        slot = sl[self.qi["pool"] % len(sl)]
        self.qi["pool"] += 1
        if slot.cnt > 0 and eng.seen.get(slot.key, 0) < slot.cnt:
            eng.e.wait_ge(slot.sem, slot.cnt)
            eng.seen[slot.key] = slot.cnt
        self._deps(eng, reads, writes, is_dma=True)
        off = bass.IndirectOffsetOnAxis(ap=idx_ap, axis=0)
        if kind == "gather":
            ins = eng.e.indirect_dma_start(out=out_ap, out_offset=None, in_=in_ap, in_offset=off)
        else:
            ins = eng.e.indirect_dma_start(out=out_ap, out_offset=off, in_=in_ap, in_offset=None)
        slot.cnt += 16
        ins.then_inc(slot.sem, 16)
        tok = (slot.key, slot.cnt, None, slot.sem)
        for b in reads:
            b.r[slot.key] = tok
        for b in writes:
            if b.w and all(t[2] is None for t in b.w) and not b.r:
                b.w = [t for t in b.w if t[0] != slot.key] + [tok]
            else:
                b.w = [tok]
            b.r = {}
        return ins

    def barrier(self):
        names = ("pe", "act", "dve", "pool", "sp")
        for xn in names:
            x = self.eng[xn]
            for yn in names:
                y = self.eng[yn]
                if yn != xn and y.cnt > 0 and x.seen.get(yn, 0) < y.cnt:
                    x.e.wait_ge(y.sem, y.cnt)
                    x.seen[yn] = y.cnt
            for q in ("sp", "pool"):
                for slot in self.slots[q]:
                    if slot.cnt > 0 and x.seen.get(slot.key, 0) < slot.cnt:
                        x.e.wait_ge(slot.sem, slot.cnt)
                        x.seen[slot.key] = slot.cnt
        self.pe_pending = set()

    def finish(self):
        eng = self.eng["sp"]
        for q in ("sp", "pool"):
            for slot in self.slots[q]:
                if slot.cnt > 0 and eng.seen.get(slot.key, 0) < slot.cnt:
                    eng.e.wait_ge(slot.sem, slot.cnt)
                    eng.seen[slot.key] = slot.cnt
        for en in ("pe", "act", "dve", "pool"):
            o = self.eng[en]
            if o.cnt > 0:
                eng.e.wait_ge(o.sem, o.cnt)


class Rot:
    def __init__(self, items):
        self.items = items
        self.i = 0

    def next(self):
        it = self.items[self.i % len(self.items)]
        self.i += 1
        return it


def build_program(nl=L, dbg=(), moe=True, stop=None):
    nc = bass.Bass("TRN2", target_bir_lowering=False)

    def din(name, shape, dt=F32):
        return nc.dram_tensor(name, list(shape), dt, kind="ExternalInput").ap()

    def dscr(name, shape, dt=F32):
        return nc.dram_tensor(name, list(shape), dt, kind="Internal").ap()

    x_in = din("x", [S, D])
    ctx_in = din("ctx", [CTX, D])
    c_pj = din("c_pj", [128, 8])
    cc_pj = din("cc_pj", [128, 8])
    w_mod = din("w_mod", [L, D, 6 * D])
    b_mod = din("b_mod", [L, 6 * D])
    norm_mix = din("norm_mix", [L, D])
    norm_ffn = din("norm_ffn", [L, D])
    w_in = din("w_in", [L, D, IN_W])
    qk_gain = din("qk_gain", [L, 768])
    sconv_w_p = din("sconv_w_p", [L, 128, 2, 3])
    conf_w_p = din("conf_w_p", [L, 128, 2, 31])
    conf_vec_p = din("conf_vec_p", [L, 128, 2, 3])
    w_out = din("w_out", [L, 1280, D])
    router_w = din("router_w", [L, D, E])
    router_b = din("router_b", [L, E])
    w_up = din("w_up", [L, E, D, 2 * D])
    bup_p = din("bup_p", [L, 128, E, 8, 2])
    w_down = din("w_down", [L, E, D, D])
    b_down = din("b_down", [L, E, D])
    final_norm = din("final_norm", [1, D])
    ident_in = din("ident", [128, 128])
    rope_cos = din("rope_cos", [S, 128])
    rope_sin = din("rope_sin", [S, 128])
    cs64_in = din("cs64", [128, 256], BF16)
    dft_in = din("dft", [NT_LAT, 128, 2, S], BF16)
    dftc_in = din("dftc", [2, 128, 2, CTX], BF16)
    U_in = din("U_bf", [128, 128], BF16)
    base8_in = din("base8", [128, 4])
    thr_in = din("thr", [1, NTM])
    out = nc.dram_tensor("out", [S, D], F32, kind="ExternalOutput").ap()
    dbg_out = {}
    for name, shape, dt_ in dbg:
        dbg_out[name] = nc.dram_tensor("dbg_" + name, list(shape), dt_, kind="ExternalOutput").ap()

    mod_d = dscr("mod_d", [L, 2, 6 * D])
    xres_d = dscr("xres_d", [NT * 128, D])
    xmid_d = dscr("xmid_d", [NT * 128, D])
    qT_d = dscr("qT_d", [128, 4, NT * 128], BF16)
    sbT_d = dscr("sbT_d", [128, 2, NT * 128], BF16)
    zl_d = dscr("zl_d", [128, 2, S + 2], BF16)
    zc_d = dscr("zc_d", [128, 2, CTX + 2], BF16)
    zcl_d = dscr("zcl_d", [128, 2, S + 30], BF16)
    zcc_d = dscr("zcc_d", [128, 2, CTX + 30], BF16)
    xcs_d = dscr("xcs_d", [NT, 128, 2, 256], BF16)
    h2tok_d = dscr("h2tok_d", [NT * 128, D], BF16)
    hs_d = dscr("hs_d", [66 * 512, D], BF16)
    ybuf_d = dscr("ybuf_d", [66 * 512, D])
    wub_d = dscr("wub_d", [E * D, 2 * D], BF16)
    wdb_d = dscr("wdb_d", [E * D, D], BF16)

    es = ExitStack()
    with es:
        T = Trk(nc, es)

        def sb(name, shape, dt=F32):
            return es.enter_context(nc.sbuf_tensor(name, list(shape), dt))

        banks = []
        for i in range(8):
            t = es.enter_context(nc.psum_tensor("ps%d" % i, [128, 512], F32))
            banks.append((t, Buf("ps%d" % i, psum=True)))

        ident = sb("ident_sb", [128, 128])
        b_ident = Buf()
        T.dma("sp", ident[:], ident_in, writes=[b_ident])
        ones_bf = sb("ones_bf", [128, 128], BF16)
        b_ones = Buf()
        T.op("dve", lambda e: e.memset(ones_bf[:], 1.0), writes=[b_ones])
        onesLN = sb("onesLN", [128, 128])
        T.op("dve", lambda e: e.memset(onesLN[:], 1.0 / 256.0), writes=[b_ones])
        epsb = sb("epsb", [128, 1])
        T.op("dve", lambda e: e.memset(epsb[:], EPS), writes=[b_ones])
        cs64 = sb("cs64_sb", [128, 256], BF16)
        T.dma("sp", cs64[:], cs64_in, writes=[b_ident])
        lgall = sb("lgall", [128, NT, E])
        m4all = sb("m4all", [128, NT, 4])
        prob4 = sb("prob4", [128, NT, 4])
        mask_bf = sb("mask_bf", [128, NT, E], BF16)
        slot4i = sb("slot4i", [128, NT, 4], I32)
        tidx = sb("tidx", [128, NTM, 4], I32)
        b_tidx = Buf()
        b_rt = [Buf() for _ in range(NT)]
        ident_bf = sb("ident_bf", [128, 128], BF16)
        U_bf = sb("U_bf_sb", [128, 128], BF16)
        T.op("act", lambda e: e.activation(out=ident_bf[:], in_=ident[:], func=AF.Copy), reads=[b_ident], writes=[b_ones])
        b_U = Buf()
        T.dma("sp", U_bf[:], U_in, writes=[b_U])
        base8 = sb("base8_sb", [128, 4])
        T.dma("sp", base8[:], base8_in, writes=[b_U])
        thr_sb = sb("thr_sb", [128, NTM])
        T.dma("sp", thr_sb[:], thr_in[0].partition_broadcast(128), writes=[b_U])
        zt = sb("zt", [128, 1, D], BF16)
        b_zt = Buf()
        T.op("pool", lambda e: e.memset(zt[:], 0.0), writes=[b_zt])
        b_hs = Buf()
        hs_v = hs_d.rearrange("(p n) c -> p n c", p=128)
        for i_ in range(264):
            T.dma("sp", hs_v[:, i_:i_ + 1, :], zt[:], reads=[b_zt], writes=[b_hs])

        zpad = sb("zpad", [128, 2, 16], BF16)
        b_zpad = Buf()
        T.op("dve", lambda e: e.memset(zpad[:], 0.0), writes=[b_zpad])
        b_pad = Buf()
        for dten, n, hp in ((zl_d, S, 1), (zc_d, CTX, 1), (zcl_d, S, 15), (zcc_d, CTX, 15)):
            T.dma("sp", dten[:, :, 0:hp], zpad[:, :, 0:hp], reads=[b_zpad], writes=[b_pad], allow_slow_non_contiguous=True)
            T.dma("sp", dten[:, :, hp + n:hp + n + hp], zpad[:, :, 0:hp], reads=[b_zpad], writes=[b_pad], allow_slow_non_contiguous=True)
        pad_bufs = []
        for _ in range(1):
            pad_bufs.append(b_pad)

        def rstd_from_ss(ss_ap, rstd_ap, n, b_ss, b_rstd, tmp_ap):
            T.op("act", lambda e: e.activation(out=tmp_ap, in_=ss_ap, func=AF.Sqrt, bias=epsb[:], scale=1.0 / n),
                 reads=[b_ss, b_ones], writes=[b_rstd])
            T.op("dve", lambda e: e.reciprocal(out=rstd_ap, in_=tmp_ap), reads=[b_rstd], writes=[b_rstd])

        supertiles = [list(range(4 * s, 4 * s + 4)) for s in range(8)] + [[32, 33]]

        b_xres = None
        for l in range(nl):
            last = (l == L - 1)
            b_wcast = Buf()
            cast_next = [0]

            cast_chunks = []
            for e_ in range(E):
                for j_ in range(8):
                    cast_chunks.append((wub_d[e_ * D + j_ * 128:e_ * D + (j_ + 1) * 128, :], w_up[l, e_, j_ * 128:(j_ + 1) * 128, :]))
                for j_ in range(4):
                    cast_chunks.append((wdb_d[e_ * D + j_ * 256:e_ * D + (j_ + 1) * 256, :], w_down[l, e_, j_ * 256:(j_ + 1) * 256, :]))

            def issue_casts(n, pace=True):
                pool, pe = T.eng["pool"], T.eng["pe"]
                for _ in range(n):
                    if cast_next[0] >= len(cast_chunks):
                        return
                    o_, i_ = cast_chunks[cast_next[0]]
                    cast_next[0] += 1
                    if pace and pe.cnt > 0 and pool.seen.get("pe", 0) < pe.cnt:
                        pool.e.wait_ge(pe.sem, pe.cnt)
                        pool.seen["pe"] = pe.cnt
                    T.dma("pool", o_, i_, writes=[b_wcast])
            x_src = (lambda t: x_in[t * 128:(t + 1) * 128, :] if t < NT_LAT else ctx_in[(t - NT_LAT) * 128:(t - NT_LAT + 1) * 128, :]) \
                if l == 0 else (lambda t: xres_d[t * 128:(t + 1) * 128, :])

            with ExitStack() as ps_:
                def sbl(name, shape, dt=F32):
                    return ps_.enter_context(nc.sbuf_tensor("%s_l%d" % (name, l), list(shape), dt))
                craw = sbl("craw", [128, 2, 8])
                cact = sbl("cact", [128, 2, 8])
                b_c = Buf()
                T.dma("sp", craw[:, 0, :], c_pj, writes=[b_c])
                T.dma("sp", craw[:, 1, :], cc_pj, writes=[b_c])
                b_cact = Buf()
                T.op("act", lambda e: e.activation(out=cact[:], in_=craw[:], func=AF.Silu), reads=[b_c], writes=[b_cact])
                modrow = sbl("modrow", [2, 6 * D])
                bmod2 = sbl("bmod2", [2, 6 * D])
                b_bm = Buf()
                T.dma("sp", bmod2[:], b_mod[l].partition_broadcast(2), writes=[b_bm])
                b_modrow = Buf()
                wms = [(sbl("wm%d" % i, [128, 8, 512]), Buf()) for i in range(2)]
                for n in range(12):
                    wm, b_wm = wms[n % 2]
                    T.dma("sp", wm[:], w_mod[l][:, n * 512:(n + 1) * 512].rearrange("(j p) f -> p j f", p=128), writes=[b_wm])
                    pt, pb = banks[n % 2]
                    for j in range(8):
                        T.mm(pt[0:2, :], cact[:, :, j], wm[:, j, :], j == 0, j == 7, [b_cact, b_wm], pb)
                    T.op("dve", lambda e: e.tensor_tensor(out=modrow[:, n * 512:(n + 1) * 512], in0=pt[0:2, :],
                                                          in1=bmod2[:, n * 512:(n + 1) * 512], op=ALU.add),
                         reads=[pb, b_bm], writes=[b_modrow])
                for a in (1, 4):
                    T.op("dve", lambda e: e.tensor_scalar(out=modrow[:, a * D:(a + 1) * D], in0=modrow[:, a * D:(a + 1) * D],
                                                          scalar1=1.0, scalar2=None, op0=ALU.add),
                         reads=[b_modrow], writes=[b_modrow])
                b_mod_d = Buf()
                T.dma("sp", mod_d[l], modrow[:], reads=[b_modrow], writes=[b_mod_d])
                T.barrier()

            if stop == "M":
                break

            def load_mod(dst, which, r, b_dst):
                T.dma("sp", dst, mod_d[l, r, which * D:(which + 1) * D].partition_broadcast(128), reads=[b_mod_d], writes=[b_dst])

            with ExitStack() as pa_:
                def sba(name, shape, dt=F32):
                    return pa_.enter_context(nc.sbuf_tensor("%s_l%d" % (name, l), list(shape), dt))
                kT = sba("kT", [128, 2, NT * 128], BF16)
                b_kT = [Buf() for _ in range(NT)]
                Vt = sba("Vt", [128, NT, 256], BF16)
                b_V = [Buf() for _ in range(NT)]
                b_qT = [Buf() for _ in range(9)]
                b_sbT = [Buf() for _ in range(9)]
                b_z = [Buf() for _ in range(9)]
                b_zc = [Buf() for _ in range(9)]
                b_xcs = [Buf() for _ in range(NT)]
                b_xmid = [Buf() for _ in range(NT)]
                b_h2tok = [Buf() for _ in range(NT)]

                with ExitStack() as p1_:
                    def sb1(name, shape, dt=F32):
                        return p1_.enter_context(nc.sbuf_tensor("%s_l%d" % (name, l), list(shape), dt))
                    w_in_bf = sb1("w_in_bf", [128, 8, IN_W], BF16)
                    b_win = Buf()
                    for j in range(8):
                        T.dma("pool", w_in_bf[:, j, :], w_in[l, j * 128:(j + 1) * 128, :], writes=[b_win], max_dma_last_dim=4096)
                    G1 = sb1("G1", [128, D])
                    sh1 = sb1("sh1", [128, D])
                    nmix = sb1("nmix", [128, D])
                    b_G1, b_sh1, b_nmix = Buf(), Buf(), Buf()
                    T.dma("sp", nmix[:], norm_mix[l].partition_broadcast(128), writes=[b_nmix])

                    def setup_mod1(r):
                        load_mod(G1[:], 1, r, b_G1)
                        load_mod(sh1[:], 0, r, b_sh1)
                        T.op("dve", lambda e: e.tensor_tensor(out=G1[:], in0=G1[:], in1=nmix[:], op=ALU.mult),
                             reads=[b_G1, b_nmix], writes=[b_G1])
                    setup_mod1(0)
                    gain = sb1("gain", [128, 768])
                    b_gain = Buf()
                    T.dma("sp", gain[:], qk_gain[l].partition_broadcast(128), writes=[b_gain])
                    T.op("dve", lambda e: e.tensor_scalar(out=gain[:, 0:512], in0=gain[:, 0:512], scalar1=128.0 ** -0.5,
                                                          scalar2=None, op0=ALU.mult), reads=[b_gain], writes=[b_gain])
                    xts = Rot([(sb1("xt%d" % i, [128, D]), Buf()) for i in range(2)])
                    hb = sb1("hb", [128, D])
                    b_hb = Buf()
                    junk = sb1("junk", [128, D])
                    b_junk = Buf()
                    ss = sb1("ss", [128, 8])
                    b_ss = Buf()
                    hTs = Rot([(sb1("hT%d" % i, [128, 8, 512], BF16), Buf()) for i in range(2)])
                    qkf = Rot([(sb1("qkf%d" % i, [128, 768]), Buf()) for i in range(2)])
                    r1 = sb1("r1", [128, 768])
                    r2 = sb1("r2", [128, 768])
                    r3 = sb1("r3", [128, 768])
                    b_r1, b_r2, b_r3 = Buf(), Buf(), Buf()
                    ss6 = sb1("ss6", [128, 24])
                    b_ss6 = Buf()
                    cst = Rot([(sb1("cst%d" % i, [128, 2, 128]), Buf()) for i in range(2)])
                    sc_sb = sb1("sc_sb", [128, 2, 512], BF16)
                    sg_sb = sb1("sg_sb", [128, 2, 512], BF16)
                    b_sc = [Buf(), Buf()]
                    b_sg = [Buf(), Buf()]
                    foT = sb1("foT", [128, 2, 512], BF16)
                    b_fo = [Buf(), Buf()]
                    stg_sb = Rot([(sb1("stg_sb%d" % i, [128, 2, 512], BF16), Buf()) for i in range(2)])
                    stg_z = Rot([(sb1("stg_z%d" % i, [128, 2, 512], BF16), Buf()) for i in range(2)])
                    stg_zc = Rot([(sb1("stg_zc%d" % i, [128, 2, 512], BF16), Buf()) for i in range(2)])
                    stg_q = Rot([(sb1("stg_q%d" % i, [128, 4, 512], BF16), Buf()) for i in range(2)])
                    stg_x = Rot([(sb1("stg_x%d" % i, [128, 2, 256], BF16), Buf()) for i in range(3)])
                    prot = Rot(banks)

                    for si, st in enumerate(supertiles):
                        isctx = (si == 8)
                        n_tok = len(st) * 128
                        if isctx:
                            setup_mod1(1)
                        hT, b_hT = hTs.next()
                        for ti, t in enumerate(st):
                            issue_casts(3)
                            xt, b_xt = xts.next()
                            rd = [b_xres[t]] if (l > 0 and b_xres) else []
                            T.dma("sp", xt[:], x_src(t), reads=rd, writes=[b_xt])
                            T.op("act", lambda e: e.activation(out=junk[:], in_=xt[:], func=AF.Square), reads=[b_xt], writes=[b_junk])
                            T.op("dve", lambda e: e.tensor_reduce(out=ss[:, 0:1], in_=junk[:], axis=AX.X, op=ALU.add),
                                 reads=[b_junk], writes=[b_ss])
                            rstd_from_ss(ss[:, 0:1], ss[:, 1:2], float(D), b_ss, b_ss, ss[:, 2:3])
                            T.op("dve", lambda e: e.scalar_tensor_tensor(out=hb[:], in0=xt[:], scalar=ss[:, 1:2], in1=G1[:],
                                                                         op0=ALU.mult, op1=ALU.mult),
                                 reads=[b_xt, b_ss, b_G1], writes=[b_hb])
                            T.op("dve", lambda e: e.tensor_tensor(out=hb[:], in0=hb[:], in1=sh1[:], op=ALU.add),
                                 reads=[b_hb, b_sh1], writes=[b_hb])
                            for half in range(2):
                                pt, pb = prot.next()
                                for jj in range(4):
                                    j = half * 4 + jj
                                    T.mm(pt[:, jj * 128:(jj + 1) * 128], hb[:, j * 128:(j + 1) * 128], ident[:], True, True,
                                         [b_hb, b_ident], pb, transpose=True)
                                T.op("act", lambda e: e.activation(out=hT[:, half * 4:half * 4 + 4, ti * 128:(ti + 1) * 128],
                                                                    in_=pt[:].rearrange("p (j t) -> p j t", j=4), func=AF.Copy),
                                     reads=[pb], writes=[b_hT])
                        sq, b_sq = stg_q.next()
                        for ti, t in enumerate(st):
                            qk, b_qk = qkf.next()
                            for half in range(2):
                                pt, pb = prot.next()
                                for j in range(8):
                                    T.mm(pt[:], hT[:, j, ti * 128:(ti + 1) * 128], w_in_bf[:, j, half * 512:(half + 1) * 512],
                                         j == 0, j == 7, [b_hT, b_win], pb)
                                if half == 0:
                                    T.op("act", lambda e: e.activation(out=qk[:, 0:512], in_=pt[:], func=AF.Copy),
                                         reads=[pb], writes=[b_qk])
                                else:
                                    T.op("act", lambda e: e.activation(out=qk[:, 512:768], in_=pt[:, 0:256], func=AF.Copy),
                                         reads=[pb], writes=[b_qk])
                                    T.op("dve", lambda e: e.tensor_copy(out=Vt[:, t, :], in_=pt[:, 256:512]),
                                         reads=[pb], writes=[b_V[t]])
                            T.op("pool", lambda e: e.tensor_tensor(out=r1[:], in0=qk[:], in1=qk[:], op=ALU.mult),
                                 reads=[b_qk], writes=[b_r1])
                            T.op("dve", lambda e: e.tensor_reduce(out=ss6[:, 0:6], in_=r1[:].rearrange("p (h d) -> p h d", h=6),
                                                                  axis=AX.X, op=ALU.add), reads=[b_r1], writes=[b_ss6])
                            rstd_from_ss(ss6[:, 0:6], ss6[:, 6:12], 128.0, b_ss6, b_ss6, ss6[:, 12:18])
                            T.op("dve", lambda e: e.tensor_tensor(out=r1[:].rearrange("p (h d) -> p h d", h=6),
                                                                  in0=qk[:].rearrange("p (h d) -> p h d", h=6),
                                                                  in1=ss6[:, 6:12].unsqueeze(2).to_broadcast([128, 6, 128]), op=ALU.mult),
                                 reads=[b_qk, b_ss6], writes=[b_r1])
                            T.op("pool", lambda e: e.tensor_tensor(out=r1[:], in0=r1[:], in1=gain[:], op=ALU.mult),
                                 reads=[b_r1, b_gain], writes=[b_r1])
                            if not isctx:
                                ct, b_ct = cst.next()
                                T.dma("sp", ct[:, 0, :], rope_cos[t * 128:(t + 1) * 128, :], writes=[b_ct])
                                T.dma("sp", ct[:, 1, :], rope_sin[t * 128:(t + 1) * 128, :], writes=[b_ct])
                                v1 = r1[:].rearrange("p (h a j i) -> p h a j i", h=6, a=2, j=2)
                                v2 = r2[:].rearrange("p (h a j i) -> p h a j i", h=6, a=2, j=2)
                                sn = ct[:, 1, :].rearrange("p (a j i) -> p a j i", a=2, j=2)
                                for jj in range(2):
                                    T.op("pool", lambda e: e.tensor_tensor(
                                        out=v2[:, :, :, jj, :], in0=v1[:, :, :, 1 - jj, :],
                                        in1=sn[:, :, jj, :].unsqueeze(1).to_broadcast([128, 6, 2, 32]), op=ALU.mult),
                                        reads=[b_r1, b_ct], writes=[b_r2])
                                T.op("dve", lambda e: e.tensor_tensor(out=r3[:].rearrange("p (h d) -> p h d", h=6),
                                                                      in0=r1[:].rearrange("p (h d) -> p h d", h=6),
                                                                      in1=ct[:, 0, :].unsqueeze(1).to_broadcast([128, 6, 128]), op=ALU.mult),
                                     reads=[b_r1, b_ct], writes=[b_r3])
                                T.op("dve", lambda e: e.tensor_tensor(out=r3[:], in0=r3[:], in1=r2[:], op=ALU.add),
                                     reads=[b_r3, b_r2], writes=[b_r3])
                                rsrc, b_rsrc = r3, b_r3
                            else:
                                rsrc, b_rsrc = r1, b_r1
                            pt, pb = prot.next()
                            for hh in range(4):
                                T.mm(pt[:, hh * 128:(hh + 1) * 128], rsrc[:, hh * 128:(hh + 1) * 128], ident[:], True, True,
                                     [b_rsrc, b_ident], pb, transpose=True)
                            T.op("act", lambda e: e.activation(out=sq[:, :, ti * 128:(ti + 1) * 128],
                                                                in_=pt[:].rearrange("p (j t) -> p j t", j=4), func=AF.Copy),
                                 reads=[pb], writes=[b_sq])
                            pt, pb = prot.next()
                            for hh in range(2):
                                T.mm(pt[:, hh * 128:(hh + 1) * 128], rsrc[:, (4 + hh) * 128:(5 + hh) * 128], ident[:], True, True,
                                     [b_rsrc, b_ident], pb, transpose=True)
                            T.op("dve", lambda e: e.tensor_copy(out=kT[:, :, t * 128:(t + 1) * 128],
                                                                in_=pt[:, 0:256].rearrange("p (j t) -> p j t", j=2)),
                                 reads=[pb], writes=[b_kT[t]])
                        t0 = st[0] * 128
                        T.dma("sp", qT_d[:, :, t0:t0 + n_tok], sq[:, :, 0:n_tok], reads=[b_sq], writes=[b_qT[si]])
                        ssb, b_ssb = stg_sb.next()
                        sz, b_sz = stg_z.next()
                        szc, b_szc = stg_zc.next()
                        order = [("sb", 0), ("sb", 1), ("sc", 0), ("sc", 1), ("sx", 0), ("sx", 1), ("fo", 0), ("fo", 1),
                                 ("g", 0), ("g", 1), ("a", 0), ("a", 1)]
                        col0 = {"sb": 1024, "sc": 1280, "sx": 1536, "fo": 1792, "a": 2048, "g": 2304}
                        for role, c in order:
                            issue_casts(1)
                            cc = col0[role] + c * 128
                            pt, pb = prot.next()
                            for j in range(8):
                                T.mm(pt[:, 0:n_tok], w_in_bf[:, j, cc:cc + 128], hT[:, j, 0:n_tok], j == 0, j == 7, [b_hT, b_win], pb)
                            if role == "sb":
                                T.op("act", lambda e: e.activation(out=ssb[:, c, 0:n_tok], in_=pt[:, 0:n_tok], func=AF.Copy),
                                     reads=[pb], writes=[b_ssb])
                            elif role == "sc":
                                T.op("act", lambda e: e.activation(out=sc_sb[:, c, 0:n_tok], in_=pt[:, 0:n_tok], func=AF.Copy),
                                     reads=[pb], writes=[b_sc[c]])
                            elif role == "sx":
                                T.op("dve", lambda e: e.tensor_tensor(out=sz[:, c, 0:n_tok], in0=pt[:, 0:n_tok], in1=sc_sb[:, c, 0:n_tok],
                                                                      op=ALU.mult), reads=[pb, b_sc[c]], writes=[b_sz])
                            elif role == "fo":
                                T.op("act", lambda e: e.activation(out=foT[:, c, 0:n_tok], in_=pt[:, 0:n_tok], func=AF.Copy),
                                     reads=[pb], writes=[b_fo[c]])
                            elif role == "g":
                                T.op("act", lambda e: e.activation(out=sg_sb[:, c, 0:n_tok], in_=pt[:, 0:n_tok], func=AF.Sigmoid),
                                     reads=[pb], writes=[b_sg[c]])
                            elif role == "a":
                                T.op("dve", lambda e: e.tensor_tensor(out=szc[:, c, 0:n_tok], in0=pt[:, 0:n_tok], in1=sg_sb[:, c, 0:n_tok],
                                                                      op=ALU.mult), reads=[pb, b_sg[c]], writes=[b_szc])
                        T.dma("sp", sbT_d[:, :, t0:t0 + n_tok], ssb[:, :, 0:n_tok], reads=[b_ssb], writes=[b_sbT[si]])
                        if not isctx:
                            T.dma("sp", zl_d[:, :, 1 + t0:1 + t0 + n_tok], sz[:, :, 0:n_tok], reads=[b_sz], writes=[b_z[si]])
                            T.dma("sp", zcl_d[:, :, 15 + t0:15 + t0 + n_tok], szc[:, :, 0:n_tok], reads=[b_szc], writes=[b_zc[si]])
                        else:
                            T.dma("sp", zc_d[:, :, 1:1 + n_tok], sz[:, :, 0:n_tok], reads=[b_sz], writes=[b_z[si]])
                            T.dma("sp", zcc_d[:, :, 15:15 + n_tok], szc[:, :, 0:n_tok], reads=[b_szc], writes=[b_zc[si]])
                        for ti, t in enumerate(st):
                            sx_, b_sx_ = stg_x.next()
                            pt, pb = prot.next()
                            for c in range(2):
                                T.mm(pt[:, c * 256:(c + 1) * 256], foT[:, c, ti * 128:(ti + 1) * 128], cs64[:], True, True,
                                     [b_fo[c], b_ident], pb)
                            T.op("dve", lambda e: e.tensor_copy(out=sx_[:], in_=pt[:].rearrange("p (c f) -> p c f", c=2)),
                                 reads=[pb], writes=[b_sx_])
                            T.dma("sp", xcs_d[t], sx_[:], reads=[b_sx_], writes=[b_xcs[t]])

                    T.barrier()
                if "kT" in dbg_out:
                    T.dma("sp", dbg_out["kT"], kT[:].rearrange("p a t -> p (a t)"), reads=b_kT)
                if "V" in dbg_out:
                    T.dma("sp", dbg_out["V"], Vt[:].rearrange("p a t -> p (a t)"), reads=b_V)

                with ExitStack() as p2_:
                    if stop == "p1":
                        break
                    def sb2(name, shape, dt=F32):
                        return p2_.enter_context(nc.sbuf_tensor("%s_l%d" % (name, l), list(shape), dt))
                    w_out_bf = sb2("w_out_bf", [128, 10, D], BF16)
                    b_wout = Buf()
                    for j in range(10):
                        T.dma("pool", w_out_bf[:, j, :], w_out[l, j * 128:(j + 1) * 128, :], writes=[b_wout], max_dma_last_dim=4096)
                    G2 = sb2("G2", [128, D])
                    sh2 = sb2("sh2", [128, D])
                    g1t = sb2("g1t", [128, D])
                    nffn = sb2("nffn", [128, D])
                    b_G2, b_sh2, b_g1, b_nffn = Buf(), Buf(), Buf(), Buf()
                    T.dma("sp", nffn[:], norm_ffn[l].partition_broadcast(128), writes=[b_nffn])

                    def setup_mod2(r):
                        load_mod(G2[:], 4, r, b_G2)
                        load_mod(sh2[:], 3, r, b_sh2)
                        load_mod(g1t[:], 2, r, b_g1)
                        T.op("dve", lambda e: e.tensor_tensor(out=G2[:], in0=G2[:], in1=nffn[:], op=ALU.mult),
                             reads=[b_G2, b_nffn], writes=[b_G2])
                    setup_mod2(0)
                    rw_sb = sb2("rw_sb", [128, 8, E])
                    b_rw = Buf()
                    T.dma("sp", rw_sb[:], router_w[l].rearrange("(j p) e -> p j e", p=128), writes=[b_rw])
                    rb_sb = sb2("rb_sb", [128, E])
                    T.dma("sp", rb_sb[:], router_b[l].partition_broadcast(128), writes=[b_rw])
                    scw = sb2("scw", [128, 2, 3])
                    cfw = sb2("cfw", [128, 2, 31])
                    cvec = sb2("cvec", [128, 2, 3])
                    b_cw = Buf()
                    T.dma("sp", scw[:], sconv_w_p[l], writes=[b_cw])
                    T.dma("sp", cfw[:], conf_w_p[l], writes=[b_cw])
                    T.dma("sp", cvec[:], conf_vec_p[l], writes=[b_cw])
                    diagS = sb2("diagS", [128, 2, 3, 128], BF16)
                    diagC = sb2("diagC", [128, 2, 31, 128], BF16)
                    b_diag = Buf()
                    for c in range(2):
                        for k in range(3):
                            T.op("dve", lambda e: e.tensor_scalar(out=diagS[:, c, k, :], in0=ident[:], scalar1=scw[:, c, k:k + 1],
                                                                  scalar2=None, op0=ALU.mult), reads=[b_ident, b_cw], writes=[b_diag])
                        for k in range(31):
                            T.op("pool", lambda e: e.tensor_scalar(out=diagC[:, c, k, :], in0=ident[:], scalar1=cfw[:, c, k:k + 1],
                                                                   scalar2=None, op0=ALU.mult), reads=[b_ident, b_cw], writes=[b_diag])
                    mixT = sb2("mixT", [128, 10, 512], BF16)
                    b_mix = [Buf() for _ in range(10)]
                    qTs = Rot([(sb2("qTs%d" % i, [128, 4, 512], BF16), Buf()) for i in range(2)])
                    PTs = Rot([(sb2("PT%d" % i, [128, 512], BF16), Buf()) for i in range(3)])
                    rden = sb2("rden", [128, 512])
                    b_rden = Buf()
                    dfts = Rot([(sb2("dft%d" % i, [128, 2, 512], BF16), Buf()) for i in range(3)])
                    xcss = Rot([(sb2("xcs%d" % i, [128, 2, 256], BF16), Buf()) for i in range(3)])
                    sbTs = sb2("sbTs", [128, 2, 512], BF16)
                    b_sbTs = Buf()
                    zs = sb2("zs", [128, 2, 514], BF16)
                    b_zs = Buf()
                    zcs = sb2("zcs", [128, 2, 542], BF16)
                    b_zcs = Buf()
                    dcs = [(sb2("dc%d" % i, [128, 512]), Buf()) for i in range(2)]
                    dsq = [(sb2("dsq%d" % i, [128, 512]), Buf()) for i in range(2)]
                    mean_sb = sb2("mean_sb", [128, 512])
                    var_sb = sb2("var_sb", [128, 512])
                    b_mean, b_var = Buf(), Buf()
                    ncs = sb2("ncs", [128, 512])
                    b_ncs = Buf()
                    xt2 = Rot([(sb2("xt2_%d" % i, [128, D]), Buf()) for i in range(2)])
                    xn2 = Rot([(sb2("xn2_%d" % i, [128, D]), Buf()) for i in range(2)])
                    h2 = sb2("h2", [128, D])
                    b_h2 = Buf()
                    junk2 = sb2("junk2", [128, D])
                    b_junk2 = Buf()
                    ss2 = sb2("ss2", [128, 8])
                    b_ss2 = Buf()
                    h2Tf = sb2("h2Tf", [128, 8, 128])
                    b_h2Tf = Buf()
                    h2bs = Rot([(sb2("h2b%d" % i, [128, D], BF16), Buf()) for i in range(2)])
                    m8 = sb2("m8", [128, 16])
                    b_m8 = Buf()

                    n_st = 9 if not last else 8
                    for si in range(n_st):
                        st = supertiles[si]
                        isctx = (si == 8)
                        n_tok = len(st) * 128
                        t0 = st[0] * 128
                        if isctx:
                            setup_mod2(1)
                        qs, b_qs = qTs.next()
                        T.dma("sp", qs[:, :, 0:n_tok], qT_d[:, :, t0:t0 + n_tok], reads=[b_qT[si]], writes=[b_qs])
                        key_tiles = [32, 33] + (list(range(32)) if not isctx else [])
                        srot = Rot(banks[0:4])
                        for h in range(4):
                            g = h // 2
                            po, bo = banks[4 + (h % 2)]
                            pd, bdn = banks[6 + (h % 2)]
                            s_list = []

                            def emit_S(ki_):
                                kt_ = key_tiles[ki_]
                                pt_, pb_ = srot.next()
                                T.mm(pt_[:, 0:n_tok], kT[:, g, kt_ * 128:(kt_ + 1) * 128], qs[:, h, 0:n_tok], True, True,
                                     [b_kT[kt_], b_qs], pb_)
                                s_list.append((pt_, pb_))
                            for ki_ in range(min(2, len(key_tiles))):
                                emit_S(ki_)
                            for ki, kt in enumerate(key_tiles):
                                if ki % 6 == 3:
                                    issue_casts(1)
                                pt, pb = s_list[ki]
                                if ki + 2 < len(key_tiles):
                                    emit_S(ki + 2)
                                PT, b_PT = PTs.next()
                                T.op("act", lambda e: e.activation(out=PT[:, 0:n_tok], in_=pt[:, 0:n_tok], func=AF.Exp),
                                     reads=[pb], writes=[b_PT])
                                fst = (ki == 0)
                                lst = (ki == len(key_tiles) - 1)
                                T.mm(po[:, 0:n_tok], Vt[:, kt, g * 128:(g + 1) * 128], PT[:, 0:n_tok], fst, lst, [b_V[kt], b_PT], bo)
                                T.mm(pd[:, 0:n_tok], ones_bf[:], PT[:, 0:n_tok], fst, lst, [b_ones, b_PT], bdn, inc=True)
                            T.op("dve", lambda e: e.reciprocal(out=rden[:, 0:n_tok], in_=pd[:, 0:n_tok]), reads=[bdn], writes=[b_rden])
                            T.op("dve", lambda e: e.tensor_tensor(out=mixT[:, h, 0:n_tok], in0=po[:, 0:n_tok], in1=rden[:, 0:n_tok],
                                                                  op=ALU.mult), reads=[bo, b_rden], writes=[b_mix[h]])
                        T.dma("sp", sbTs[:, :, 0:n_tok], sbT_d[:, :, t0:t0 + n_tok], reads=[b_sbT[si]], writes=[b_sbTs])
                        nb = [s2 for s2 in (si - 1, si, si + 1) if 0 <= s2 < 8] if not isctx else [8]
                        if not isctx:
                            T.dma("sp", zs[:, :, 0:n_tok + 2], zl_d[:, :, t0:t0 + n_tok + 2], reads=[b_z[s2] for s2 in nb] + [b_pad], writes=[b_zs])
                            T.dma("sp", zcs[:, :, 0:n_tok + 30], zcl_d[:, :, t0:t0 + n_tok + 30], reads=[b_zc[s2] for s2 in nb] + [b_pad], writes=[b_zcs])
                        else:
                            T.dma("sp", zs[:, :, 0:n_tok + 2], zc_d[:, :, 0:n_tok + 2], reads=[b_z[8], b_pad], writes=[b_zs])
                            T.dma("sp", zcs[:, :, 0:n_tok + 30], zcc_d[:, :, 0:n_tok + 30], reads=[b_zc[8], b_pad], writes=[b_zcs])
                        prot2 = Rot(banks[0:4])
                        for c in range(2):
                            pt, pb = prot2.next()
                            for k in range(3):
                                T.mm(pt[:, 0:n_tok], diagS[:, c, k, :], zs[:, c, k:k + n_tok], k == 0, k == 2, [b_diag, b_zs], pb)
                            T.op("dve", lambda e: e.tensor_tensor(out=mixT[:, 4 + c, 0:n_tok], in0=pt[:, 0:n_tok], in1=sbTs[:, c, 0:n_tok],
                                                                  op=ALU.mult), reads=[pb, b_sbTs], writes=[b_mix[4 + c]])
                        for c in range(2):
                            pt, pb = prot2.next()
                            for k in range(31):
                                T.mm(pt[:, 0:n_tok], diagC[:, c, k, :], zcs[:, c, k:k + n_tok], k == 0, k == 30, [b_diag, b_zcs], pb)
                            dc, b_dc = dcs[c]
                            dq, b_dq = dsq[c]
                            T.op("act", lambda e: e.activation(out=dc[:, 0:n_tok], in_=pt[:, 0:n_tok], func=AF.Identity,
                                                                bias=cvec[:, c, 0:1], scale=1.0), reads=[pb, b_cw], writes=[b_dc])
                            T.op("pool", lambda e: e.tensor_tensor(out=dq[:, 0:n_tok], in0=dc[:, 0:n_tok], in1=dc[:, 0:n_tok], op=ALU.mult),
                                 reads=[b_dc], writes=[b_dq])
                        pm, bm = banks[4]
                        pq, bq = banks[5]
                        for c in range(2):
                            T.mm(pm[:, 0:n_tok], onesLN[:], dcs[c][0][:, 0:n_tok], c == 0, c == 1, [b_ones, dcs[c][1]], bm)
                        for c in range(2):
                            T.mm(pq[:, 0:n_tok], onesLN[:], dsq[c][0][:, 0:n_tok], c == 0, c == 1, [b_ones, dsq[c][1]], bq)
                        T.op("act", lambda e: e.activation(out=mean_sb[:, 0:n_tok], in_=pm[:, 0:n_tok], func=AF.Copy), reads=[bm], writes=[b_mean])
                        T.op("pool", lambda e: e.tensor_tensor(out=var_sb[:, 0:n_tok], in0=mean_sb[:, 0:n_tok], in1=mean_sb[:, 0:n_tok], op=ALU.mult),
                             reads=[b_mean], writes=[b_var])
                        T.op("dve", lambda e: e.tensor_tensor(out=var_sb[:, 0:n_tok], in0=pq[:, 0:n_tok], in1=var_sb[:, 0:n_tok], op=ALU.subtract),
                             reads=[bq, b_var], writes=[b_var])
                        T.op("act", lambda e: e.activation(out=var_sb[:, 0:n_tok], in_=var_sb[:, 0:n_tok], func=AF.Sqrt, bias=epsb[:], scale=1.0),
                             reads=[b_var, b_ones], writes=[b_var])
                        T.op("dve", lambda e: e.reciprocal(out=var_sb[:, 0:n_tok], in_=var_sb[:, 0:n_tok]), reads=[b_var], writes=[b_var])
                        for c in range(2):
                            dc, b_dc = dcs[c]
                            T.op("dve", lambda e: e.tensor_tensor(out=ncs[:, 0:n_tok], in0=dc[:, 0:n_tok], in1=mean_sb[:, 0:n_tok], op=ALU.subtract),
                                 reads=[b_dc, b_mean], writes=[b_ncs])
                            T.op("dve", lambda e: e.tensor_tensor(out=ncs[:, 0:n_tok], in0=ncs[:, 0:n_tok], in1=var_sb[:, 0:n_tok], op=ALU.mult),
                                 reads=[b_ncs, b_var], writes=[b_ncs])
                            T.op("act", lambda e: e.activation(out=mixT[:, 8 + c, 0:n_tok], in_=ncs[:, 0:n_tok], func=AF.Silu,
                                                                bias=cvec[:, c, 2:3], scale=cvec[:, c, 1:2]),
                                 reads=[b_ncs, b_cw], writes=[b_mix[8 + c]])
                        pf = [banks[6], banks[7]]
                        ftiles = list(range(32)) if not isctx else [32, 33]
                        for fi, ft in enumerate(ftiles):
                            dt_, b_dt = dfts.next()
                            if not isctx:
                                T.dma("sp", dt_[:, :, 0:n_tok], dft_in[ft, :, :, t0:t0 + n_tok], writes=[b_dt])
                            else:
                                T.dma("sp", dt_[:, :, 0:n_tok], dftc_in[ft - 32], writes=[b_dt])
                            xc_, b_xc = xcss.next()
                            T.dma("sp", xc_[:], xcs_d[ft], reads=[b_xcs[ft]], writes=[b_xc])
                            for c in range(2):
                                pt, pb = pf[c]
                                T.mm(pt[:, 0:n_tok], xc_[:, c, 0:128], dt_[:, 0, 0:n_tok], fi == 0, False, [b_xc, b_dt], pb)
                                T.mm(pt[:, 0:n_tok], xc_[:, c, 128:256], dt_[:, 1, 0:n_tok], False, fi == len(ftiles) - 1, [b_xc, b_dt], pb,
                                     inc=(c == 1))
                        for c in range(2):
                            pt, pb = pf[c]
                            T.op("act", lambda e: e.activation(out=mixT[:, 6 + c, 0:n_tok], in_=pt[:, 0:n_tok], func=AF.Copy),
                                 reads=[pb], writes=[b_mix[6 + c]])
                        if "mixT" in dbg_out and si == 0:
                            T.dma("sp", dbg_out["mixT"], mixT[:].rearrange("p a t -> p (a t)"), reads=b_mix)
                        prot3 = Rot(banks[0:4])
                        for ti, t in enumerate(st):
                            xt, b_xt = xt2.next()
                            rd = [b_xres[t]] if (l > 0 and b_xres) else []
                            T.dma("sp", xt[:], x_src(t), reads=rd, writes=[b_xt])
                            xn, b_xn = xn2.next()
                            for dh in range(2):
                                pt, pb = prot3.next()
                                for kc in range(10):
                                    T.mm(pt[:], mixT[:, kc, ti * 128:(ti + 1) * 128], w_out_bf[:, kc, dh * 512:(dh + 1) * 512],
                                         kc == 0, kc == 9, [b_mix[kc], b_wout], pb)
                                T.op("dve", lambda e: e.tensor_tensor(out=xn[:, dh * 512:(dh + 1) * 512], in0=pt[:],
                                                                      in1=g1t[:, dh * 512:(dh + 1) * 512], op=ALU.mult),
                                     reads=[pb, b_g1], writes=[b_xn])
                            T.op("pool", lambda e: e.tensor_tensor(out=xn[:], in0=xn[:], in1=xt[:], op=ALU.add),
                                 reads=[b_xn, b_xt], writes=[b_xn])
                            T.dma("sp", xmid_d[t * 128:(t + 1) * 128, :], xn[:], reads=[b_xn], writes=[b_xmid[t]])
                            T.op("act", lambda e: e.activation(out=junk2[:], in_=xn[:], func=AF.Square), reads=[b_xn], writes=[b_junk2])
                            T.op("dve", lambda e: e.tensor_reduce(out=ss2[:, 0:1], in_=junk2[:], axis=AX.X, op=ALU.add),
                                 reads=[b_junk2], writes=[b_ss2])
                            rstd_from_ss(ss2[:, 0:1], ss2[:, 1:2], float(D), b_ss2, b_ss2, ss2[:, 2:3])
                            T.op("dve", lambda e: e.scalar_tensor_tensor(out=h2[:], in0=xn[:], scalar=ss2[:, 1:2], in1=G2[:],
                                                                         op0=ALU.mult, op1=ALU.mult),
                                 reads=[b_xn, b_ss2, b_G2], writes=[b_h2])
                            T.op("pool", lambda e: e.tensor_tensor(out=h2[:], in0=h2[:], in1=sh2[:], op=ALU.add),
                                 reads=[b_h2, b_sh2], writes=[b_h2])
                            h2b, b_h2b = h2bs.next()
                            T.op("act", lambda e: e.activation(out=h2b[:], in_=h2[:], func=AF.Copy), reads=[b_h2], writes=[b_h2b])
                            T.dma("sp", h2tok_d[t * 128:(t + 1) * 128, :], h2b[:], reads=[b_h2b], writes=[b_h2tok[t]])
                            for half in range(2):
                                pt, pb = prot3.next()
                                for jj in range(4):
                                    j = half * 4 + jj
                                    T.mm(pt[:, jj * 128:(jj + 1) * 128], h2[:, j * 128:(j + 1) * 128], ident[:], True, True,
                                         [b_h2, b_ident], pb, transpose=True)
                                T.op("act", lambda e: e.activation(out=h2Tf[:, half * 4:half * 4 + 4, :],
                                                                    in_=pt[:].rearrange("p (j t) -> p j t", j=4), func=AF.Copy),
                                     reads=[pb], writes=[b_h2Tf])
                            pt, pb = prot3.next()
                            for j in range(8):
                                T.mm(pt[:, 0:E], h2Tf[:, j, :], rw_sb[:, j, :], j == 0, j == 7, [b_h2Tf, b_rw], pb)
                            T.op("dve", lambda e: e.tensor_tensor(out=lgall[:, t, :], in0=pt[:, 0:E], in1=rb_sb[:], op=ALU.add),
                                 reads=[pb, b_rw], writes=[b_rt[t]])
                            T.op("dve", lambda e: e.max(out=m8[:, 0:8], in_=lgall[:, t, :]), reads=[b_rt[t]], writes=[b_m8])
                            T.op("dve", lambda e: e.tensor_scalar(out=mask_bf[:, t, :], in0=lgall[:, t, :], scalar1=m8[:, 3:4], scalar2=None,
                                                                  op0=ALU.is_ge), reads=[b_rt[t], b_m8], writes=[b_rt[t]])
                            T.op("dve", lambda e: e.tensor_copy(out=m4all[:, t, :], in_=m8[:, 0:4]), reads=[b_m8], writes=[b_rt[t]])
                            T.op("dve", lambda e: e.tensor_scalar(out=m8[:, 8:9], in0=m8[:, 0:1], scalar1=-1.0, scalar2=None, op0=ALU.mult),
                                 reads=[b_m8], writes=[b_m8])
                            T.op("act", lambda e: e.activation(out=m8[:, 12:16], in_=m8[:, 0:4], func=AF.Exp, bias=m8[:, 8:9], scale=1.0),
                                 reads=[b_m8], writes=[b_m8])
                            T.op("dve", lambda e: e.tensor_reduce(out=m8[:, 9:10], in_=m8[:, 12:16], axis=AX.X, op=ALU.add),
                                 reads=[b_m8], writes=[b_m8])
                            T.op("dve", lambda e: e.reciprocal(out=m8[:, 10:11], in_=m8[:, 9:10]), reads=[b_m8], writes=[b_m8])
                            T.op("dve", lambda e: e.tensor_scalar(out=prob4[:, t, :], in0=m8[:, 12:16], scalar1=m8[:, 10:11], scalar2=None,
                                                                  op0=ALU.mult), reads=[b_m8], writes=[b_rt[t]])

                    T.barrier()
                T.barrier()
            if "xmid" in dbg_out and l == 0:
                T.dma("sp", dbg_out["xmid"], xmid_d, reads=b_xmid)
                T.barrier()

            n_tiles = NT if not last else NT_LAT
            NTILE = (4 * n_tiles * 128) // TS + 32
            issue_casts(len(cast_chunks), pace=False)
            b_xres_new = [Buf() for _ in range(NT)]
            b_slot = Buf()
            if moe:
                with ExitStack() as pr_:
                    def sbr(name, shape, dt=F32):
                        return pr_.enter_context(nc.sbuf_tensor("%s_l%d" % (name, l), list(shape), dt))
                    rank_all = sbr("rank_all", [128, NT, E])
                    b_rank = Buf()
                    rrot = Rot(banks[0:6])
                    for t in range(n_tiles):
                        pt, pb = rrot.next()
                        for t2 in range(t):
                            T.mm(pt[:, 0:E], ones_bf[:], mask_bf[:, t2, :], t2 == 0, False, [b_ones, b_rt[t2]], pb)
                        T.mm(pt[:, 0:E], U_bf[:], mask_bf[:, t, :], t == 0, True, [b_U, b_rt[t]], pb)
                        T.op("act", lambda e: e.activation(out=rank_all[:, t, :], in_=pt[:, 0:E], func=AF.Copy), reads=[pb], writes=[b_rank])
                    cnt = sbr("cnt", [128, E])
                    ntl = sbr("ntl", [128, E])
                    offs = sbr("offs", [128, E + 1])
                    b_cnt = Buf()
                    pt, pb = banks[6]
                    for t in range(n_tiles):
                        T.mm(pt[:, 0:E], ones_bf[:], mask_bf[:, t, :], t == 0, t == n_tiles - 1, [b_ones, b_rt[t]], pb)
                    T.op("dve", lambda e: e.tensor_copy(out=cnt[:], in_=pt[:, 0:E]), reads=[pb], writes=[b_cnt])
                    T.op("dve", lambda e: e.memset(ntl[:], 0.0), writes=[b_cnt])
                    for j in range((NT * 128) // TS + 1):
                        T.op("dve", lambda e: e.scalar_tensor_tensor(out=ntl[:], in0=cnt[:], scalar=float(TS) * j, in1=ntl[:],
                                                                     op0=ALU.is_gt, op1=ALU.add), reads=[b_cnt], writes=[b_cnt])
                    T.op("dve", lambda e: e.memset(offs[:], 0.0), writes=[b_cnt])
                    for e_ in range(E):
                        T.op("dve", lambda e: e.scalar_tensor_tensor(out=offs[:, e_ + 1:e_ + 2], in0=ntl[:, e_:e_ + 1], scalar=float(TS),
                                                                     in1=offs[:, e_:e_ + 1], op0=ALU.mult, op1=ALU.add),
                             reads=[b_cnt], writes=[b_cnt])
                    T.op("dve", lambda e: e.tensor_tensor(out=rank_all[:, 0:n_tiles, :], in0=rank_all[:, 0:n_tiles, :],
                                                          in1=offs[:, 0:E].unsqueeze(1).to_broadcast([128, n_tiles, E]), op=ALU.add),
                         reads=[b_rank, b_cnt], writes=[b_rank])
                    oh = sbr("oh", [128, NT, E])
                    slot4f = sbr("slot4f", [128, NT, 4])
                    b_oh = Buf()
                    for k in range(4):
                        T.op("dve", lambda e: e.tensor_tensor(out=oh[:, 0:n_tiles, :], in0=lgall[:, 0:n_tiles, :],
                                                              in1=m4all[:, 0:n_tiles, k:k + 1].to_broadcast([128, n_tiles, E]), op=ALU.is_equal),
                             reads=b_rt[0:n_tiles], writes=[b_oh])
                        T.op("dve", lambda e: e.tensor_tensor(out=oh[:, 0:n_tiles, :], in0=oh[:, 0:n_tiles, :], in1=rank_all[:, 0:n_tiles, :],
                                                              op=ALU.mult), reads=[b_oh, b_rank], writes=[b_oh])
                        T.op("dve", lambda e: e.tensor_reduce(out=slot4f[:, 0:n_tiles, k], in_=oh[:, 0:n_tiles, :], axis=AX.X, op=ALU.add),
                             reads=[b_oh], writes=[b_oh])
                    T.op("dve", lambda e: e.tensor_copy(out=slot4i[:, 0:n_tiles, :], in_=slot4f[:, 0:n_tiles, :]), reads=[b_oh], writes=[b_slot])
                    te = sbr("te", [128, NTM])
                    tidx_f = sbr("tidx_f", [128, NTM, 4])
                    b_te = Buf()
                    T.op("dve", lambda e: e.memset(te[:], 0.0), writes=[b_te])
                    for e_ in range(E):
                        T.op("dve", lambda e: e.scalar_tensor_tensor(out=te[:], in0=thr_sb[:], scalar=offs[:, e_ + 1:e_ + 2], in1=te[:],
                                                                     op0=ALU.is_ge, op1=ALU.add), reads=[b_cnt, b_U, b_te], writes=[b_te])
                    T.op("dve", lambda e: e.tensor_scalar(out=te[:], in0=te[:], scalar1=float(E - 1), scalar2=None, op0=ALU.min),
                         reads=[b_te], writes=[b_te])
                    T.op("dve", lambda e: e.tensor_scalar(out=tidx_f[:, :, 0], in0=te[:], scalar1=128.0, scalar2=base8[:, 0:1],
                                                          op0=ALU.mult, op1=ALU.add), reads=[b_te, b_U], writes=[b_te])
                    T.op("dve", lambda e: e.tensor_scalar(out=tidx_f[:, :, 1], in0=te[:], scalar1=base8[:, 1:2], scalar2=float(l * 4096),
                                                          op0=ALU.add, op1=ALU.add), reads=[b_te, b_U], writes=[b_te])
                    T.op("dve", lambda e: e.tensor_scalar(out=tidx_f[:, :, 2], in0=te[:], scalar1=float(l * 32), scalar2=None,
                                                          op0=ALU.add), reads=[b_te], writes=[b_te])
                    T.op("dve", lambda e: e.tensor_copy(out=tidx_f[:, :, 3], in_=te[:]), reads=[b_te], writes=[b_te])
                    T.op("dve", lambda e: e.tensor_copy(out=tidx[:], in_=tidx_f[:]), reads=[b_te], writes=[b_tidx])
                    hrs = Rot([(sbr("hrow%d" % i, [128, D], BF16), Buf()) for i in range(3)])
                    for t in range(n_tiles):
                        hr, b_hr = hrs.next()
                        T.dma("sp", hr[:], h2tok_d[t * 128:(t + 1) * 128, :], reads=[b_h2tok[t]], writes=[b_hr])
                        for k in range(4):
                            T.idma("scatter", hs_d, slot4i[:, t, k:k + 1], hr[:], reads=[b_hr, b_slot], writes=[b_hs])
                    if "slot4" in dbg_out:
                        T.dma("sp", dbg_out["slot4"], slot4f[:].rearrange("p a t -> p (a t)"), reads=[b_oh])
                        T.dma("sp", dbg_out["te"], te[:], reads=[b_te])
                    T.barrier()

                b_ybuf = Buf()
                with ExitStack() as pb_:
                    def sbm(name, shape, dt=F32):
                        return pb_.enter_context(nc.sbuf_tensor("%s_l%d" % (name, l), list(shape), dt))
                    wus = Rot([(sbm("wu%d" % i, [128, 8, 2 * D], BF16), Buf()) for i in range(3)])
                    wds = Rot([(sbm("wd%d" % i, [128, 8, D], BF16), Buf()) for i in range(3)])
                    hsts = Rot([(sbm("hst%d" % i, [128, TS // 128, D], BF16), Buf()) for i in range(3)])
                    bupts = Rot([(sbm("bupt%d" % i, [128, 8, 2]), Buf()) for i in range(3)])
                    bdts = Rot([(sbm("bdt%d" % i, [128, D]), Buf()) for i in range(2)])
                    hTs2 = Rot([(sbm("hTm%d" % i, [128, 8, TS], BF16), Buf()) for i in range(2)])
                    actT = Rot([(sbm("actT%d" % i, [128, 8, TS], BF16), Buf()) for i in range(2)])
                    gs = Rot([(sbm("g_sb%d" % i, [128, TS]), Buf()) for i in range(2)])
                    ls = Rot([(sbm("l_sb%d" % i, [128, TS]), Buf()) for i in range(2)])
                    ysbs = Rot([(sbm("ysb%d" % i, [128, D]), Buf()) for i in range(2)])
                    w_up_rows = wub_d.rearrange("(b j) f -> b (j f)", j=8)
                    w_dn_rows = wdb_d.rearrange("(b j) f -> b (j f)", j=8)
                    bup_rows = bup_p.rearrange("l p e c t -> (l p e) (c t)")
                    bdn_rows = b_down.rearrange("l e d -> (l e) d")
                    tq = []

                    def issue_tile(i):
                        wu, b_wu = wus.next()
                        wd, b_wd = wds.next()
                        hst, b_hst = hsts.next()
                        bupt, b_bupt = bupts.next()
                        bdt, b_bdt = bdts.next()
                        T.dma("sp", hst[:], hs_d[i * TS:(i + 1) * TS, :].rearrange("(s p) c -> p s c", p=128), reads=[b_hs], writes=[b_hst])
                        T.idma("gather", wu[:].rearrange("p j f -> p (j f)"), tidx[:, i, 0:1], w_up_rows, reads=[b_tidx, b_wcast], writes=[b_wu])
                        T.idma("gather", wd[:].rearrange("p j f -> p (j f)"), tidx[:, i, 0:1], w_dn_rows, reads=[b_tidx, b_wcast], writes=[b_wd])
                        T.idma("gather", bupt[:].rearrange("p c t -> p (c t)"), tidx[:, i, 1:2], bup_rows, reads=[b_tidx], writes=[b_bupt])
                        T.idma("gather", bdt[:], tidx[:, i, 2:3], bdn_rows, reads=[b_tidx], writes=[b_bdt])
                        tq.append((wu, b_wu, wd, b_wd, hst, b_hst, bupt, b_bupt, bdt, b_bdt))
                    issue_tile(0)
                    issue_tile(1)
                    up_rot = Rot(banks[0:4])
                    tr_rot = Rot(banks[4:6])
                    dn_rot = Rot(banks[6:8])
                    for i in range(NTILE):
                        wu, b_wu, wd, b_wd, hst, b_hst, bupt, b_bupt, bdt, b_bdt = tq.pop(0)
                        if i + 2 < NTILE:
                            issue_tile(i + 2)
                        T.op("dve", lambda e: e.tensor_scalar(out=bupt[:, :, 1], in0=bupt[:, :, 1], scalar1=1.0, scalar2=None, op0=ALU.add),
                             reads=[b_bupt], writes=[b_bupt])
                        hT, b_hT = hTs2.next()
                        for s_ in range(TS // 128):
                            pt, pbk = tr_rot.next()
                            ptb = pt[:].bitcast(BF16)
                            for j in range(8):
                                T.mm(ptb[:, j * 128:(j + 1) * 128], hst[:, s_, j:D:8], ident_bf[:], True, True,
                                     [b_hst, b_ones], pbk, transpose=True)
                            T.op("act", lambda e: e.activation(out=hT[:, :, s_ * 128:(s_ + 1) * 128],
                                                                in_=ptb.rearrange("p (j t) -> p j t", j=8), func=AF.Copy),
                                 reads=[pbk], writes=[b_hT])
                        n = TS
                        aT, b_aT = actT.next()
                        for c in range(8):
                            pg, bg = up_rot.next()
                            pl, bl = up_rot.next()
                            for j in range(8):
                                T.mm(pg[:, 0:n], wu[:, j, 2 * c:2 * D:16], hT[:, j, 0:n], j == 0, j == 7, [b_wu, b_hT], bg)
                            for j in range(8):
                                T.mm(pl[:, 0:n], wu[:, j, 2 * c + 1:2 * D:16], hT[:, j, 0:n], j == 0, j == 7, [b_wu, b_hT], bl)
                            g_sb, b_g = gs.next()
                            l_sb, b_l = ls.next()
                            T.op("dve", lambda e: e.tensor_scalar(out=g_sb[:, 0:n], in0=pg[:, 0:n], scalar1=bupt[:, c, 0:1], scalar2=7.0,
                                                                  op0=ALU.add, op1=ALU.min), reads=[bg, b_bupt], writes=[b_g])
                            T.op("act", lambda e: e.activation(out=g_sb[:, 0:n], in_=g_sb[:, 0:n], func=AF.Gelu_apprx_sigmoid),
                                 reads=[b_g], writes=[b_g])
                            T.op("dve", lambda e: e.tensor_scalar(out=l_sb[:, 0:n], in0=pl[:, 0:n], scalar1=bupt[:, c, 1:2], scalar2=8.0,
                                                                  op0=ALU.add, op1=ALU.min), reads=[bl, b_bupt], writes=[b_l])
                            T.op("dve", lambda e: e.scalar_tensor_tensor(out=aT[:, c, 0:n], in0=l_sb[:, 0:n], scalar=-6.0, in1=g_sb[:, 0:n],
                                                                         op0=ALU.max, op1=ALU.mult), reads=[b_g, b_l], writes=[b_aT])
                        for s_ in range(TS // 128):
                            ysb, b_ysb = ysbs.next()
                            for dh in range(2):
                                pt, pbk = dn_rot.next()
                                for c in range(8):
                                    T.mm(pt[:], aT[:, c, s_ * 128:(s_ + 1) * 128], wd[:, c, dh * 512:(dh + 1) * 512],
                                         c == 0, c == 7, [b_aT, b_wd], pbk)
                                T.op("dve", lambda e: e.tensor_tensor(out=ysb[:, dh * 512:(dh + 1) * 512], in0=pt[:],
                                                                      in1=bdt[:, dh * 512:(dh + 1) * 512], op=ALU.add),
                                     reads=[pbk, b_bdt], writes=[b_ysb])
                            r0 = i * TS + s_ * 128
                            T.dma("sp", ybuf_d[r0:r0 + 128, :], ysb[:], reads=[b_ysb], writes=[b_ybuf])
                    T.barrier()

                with ExitStack() as pc_:
                    def sbc(name, shape, dt=F32):
                        return pc_.enter_context(nc.sbuf_tensor("%s_l%d" % (name, l), list(shape), dt))
                    yks = Rot([(sbc("yk%d" % i, [128, D]), Buf()) for i in range(8)])
                    accs = Rot([(sbc("acc%d" % i, [128, D]), Buf()) for i in range(2)])
                    xm = Rot([(sbc("xm%d" % i, [128, D]), Buf()) for i in range(2)])
                    g2t = sbc("g2t", [128, D])
                    b_g2 = Buf()
                    fn_t = sbc("fn_t", [128, D])
                    b_fn = Buf()
                    if last:
                        T.dma("sp", fn_t[:], final_norm[0].partition_broadcast(128), writes=[b_fn])
                    junk3 = sbc("junk3", [128, D])
                    b_junk3 = Buf()
                    ss3 = sbc("ss3", [128, 8])
                    b_ss3 = Buf()
                    cur_r = [None]

                    def set_g2(r):
                        if cur_r[0] != r:
                            load_mod(g2t[:], 5, r, b_g2)
                            cur_r[0] = r
                    for t in range(n_tiles):
                        set_g2(0 if t < NT_LAT else 1)
                        xmt, b_xm = xm.next()
                        T.dma("sp", xmt[:], xmid_d[t * 128:(t + 1) * 128, :], reads=[b_xmid[t]], writes=[b_xm])
                        acc, b_acc = accs.next()
                        for k in range(4):
                            yk, b_yk = yks.next()
                            T.idma("gather", yk[:], slot4i[:, t, k:k + 1], ybuf_d, reads=[b_slot, b_ybuf], writes=[b_yk])
                            if k == 0:
                                T.op("dve", lambda e: e.tensor_scalar(out=acc[:], in0=yk[:], scalar1=prob4[:, t, 0:1], scalar2=None, op0=ALU.mult),
                                     reads=[b_yk, b_rt[t]], writes=[b_acc])
                            else:
                                T.op("dve", lambda e: e.scalar_tensor_tensor(out=acc[:], in0=yk[:], scalar=prob4[:, t, k:k + 1], in1=acc[:],
                                                                             op0=ALU.mult, op1=ALU.add), reads=[b_yk, b_rt[t], b_acc], writes=[b_acc])
                        T.op("dve", lambda e: e.tensor_tensor(out=acc[:], in0=acc[:], in1=g2t[:], op=ALU.mult),
                             reads=[b_acc, b_g2], writes=[b_acc])
                        T.op("dve", lambda e: e.tensor_tensor(out=xmt[:], in0=xmt[:], in1=acc[:], op=ALU.add),
                             reads=[b_xm, b_acc], writes=[b_xm])
                        if not last:
                            T.dma("sp", xres_d[t * 128:(t + 1) * 128, :], xmt[:], reads=[b_xm], writes=[b_xres_new[t]])
                        else:
                            T.op("act", lambda e: e.activation(out=junk3[:], in_=xmt[:], func=AF.Square), reads=[b_xm], writes=[b_junk3])
                            T.op("dve", lambda e: e.tensor_reduce(out=ss3[:, 0:1], in_=junk3[:], axis=AX.X, op=ALU.add),
                                 reads=[b_junk3], writes=[b_ss3])
                            rstd_from_ss(ss3[:, 0:1], ss3[:, 1:2], float(D), b_ss3, b_ss3, ss3[:, 2:3])
                            T.op("dve", lambda e: e.scalar_tensor_tensor(out=xmt[:], in0=xmt[:], scalar=ss3[:, 1:2], in1=fn_t[:],
                                                                         op0=ALU.mult, op1=ALU.mult),
                                 reads=[b_xm, b_ss3, b_fn], writes=[b_xm])
                            T.dma("sp", out[t * 128:(t + 1) * 128, :], xmt[:], reads=[b_xm])
            b_xres = b_xres_new
            T.barrier()
        T.finish()
    return nc


def _consts():
    ident = np.eye(128, dtype=np.float32)
    rows = S // 64
    row = np.broadcast_to(np.arange(rows, dtype=np.float32)[:, None], (rows, 64)).reshape(-1)
    col = np.broadcast_to(np.arange(64, dtype=np.float32)[None, :], (rows, 64)).reshape(-1)
    inv_freq = (10000.0 ** (-np.arange(32, dtype=np.float32) * 2.0 / 64)).astype(np.float32)
    ar = row[:, None] * inv_freq
    ac = col[:, None] * inv_freq
    ang = np.concatenate([ar, ar, ac, ac], axis=-1).astype(np.float32)
    cos = np.cos(ang).astype(np.float32)
    sin = np.sin(ang).astype(np.float32)
    sgn = np.tile(np.concatenate([-np.ones(32), np.ones(32)]), 2).astype(np.float32)
    sinS = (sin * sgn[None, :]).astype(np.float32)
    k = np.arange(64)
    a64 = 2 * np.pi * np.outer(k, k) / 64.0
    c64, s64 = np.cos(a64), np.sin(a64)
    cs = np.zeros((128, 256), np.float64)
    for g in range(2):
        cs[g * 64:(g + 1) * 64, g * 64:(g + 1) * 64] = c64
        cs[g * 64:(g + 1) * 64, 128 + g * 64:128 + (g + 1) * 64] = s64
    t = np.arange(S, dtype=np.int64)
    m = (np.outer(t, t) % S).astype(np.float64)
    a = 2 * np.pi * m / S
    dft = np.empty((NT_LAT, 128, 2, S), dtype=ml_dtypes.bfloat16)
    dft[:, :, 0, :] = (np.cos(a) / 512.0).reshape(NT_LAT, 128, S).astype(ml_dtypes.bfloat16)
    dft[:, :, 1, :] = (-np.sin(a) / 512.0).reshape(NT_LAT, 128, S).astype(ml_dtypes.bfloat16)
    tc = np.arange(CTX, dtype=np.int64)
    ac_ = 2 * np.pi * (np.outer(tc, tc) % CTX).astype(np.float64) / CTX
    dftc = np.empty((2, 128, 2, CTX), dtype=ml_dtypes.bfloat16)
    dftc[:, :, 0, :] = (np.cos(ac_) / 128.0).reshape(2, 128, CTX).astype(ml_dtypes.bfloat16)
    dftc[:, :, 1, :] = (-np.sin(ac_) / 128.0).reshape(2, 128, CTX).astype(ml_dtypes.bfloat16)
    U = (np.arange(128)[:, None] < np.arange(128)[None, :]).astype(np.float32).astype(ml_dtypes.bfloat16)
    base8 = np.zeros((128, 4), np.float32)
    base8[:, 0] = np.arange(128)
    base8[:, 1] = np.arange(128) * 32
    thr = (float(TS) * np.arange(NTM, dtype=np.float32)).reshape(1, NTM)
    return dict(ident=ident, rope_cos=cos, rope_sin=sinS, cs64=cs.astype(ml_dtypes.bfloat16), dft=dft, dftc=dftc,
                U_bf=U, base8=base8, thr=thr)


def _shared_inputs(inp):
    f = lambda a: np.ascontiguousarray(np.asarray(a, dtype=np.float32))
    sh = {}
    for k in ("w_mod", "b_mod", "norm_mix", "norm_ffn", "w_in", "w_out", "router_w", "router_b", "w_up", "w_down", "b_down"):
        sh[k] = f(inp[k])
    sh["final_norm"] = f(inp["final_norm"]).reshape(1, D)
    qn, kn = f(inp["q_norm"]), f(inp["k_norm"])
    sh["qk_gain"] = np.ascontiguousarray(np.concatenate([np.tile(qn, (1, 4)), np.tile(kn, (1, 2))], axis=1))
    sh["sconv_w_p"] = np.ascontiguousarray(f(inp["sconv_w"]).reshape(L, 3, 2, 128).transpose(0, 3, 2, 1))
    sh["conf_w_p"] = np.ascontiguousarray(f(inp["conf_dw_w"]).reshape(L, 31, 2, 128).transpose(0, 3, 2, 1))
    cv = np.stack([f(inp["conf_dw_b"]), f(inp["conf_ln_g"]), f(inp["conf_ln_b"])], axis=1)
    sh["conf_vec_p"] = np.ascontiguousarray(cv.reshape(L, 3, 2, 128).transpose(0, 3, 2, 1))
    sh["bup_p"] = np.ascontiguousarray(f(inp["b_up"]).reshape(L, E, 128, 8, 2).transpose(0, 2, 1, 3, 4))
    sh["cc_pj"] = np.ascontiguousarray(f(inp["c_ctx"]).reshape(8, 128).T)
    sh.update(_consts())
    return sh


def _core_inputs(inp, sh, b):
    m = dict(sh)
    m["x"] = np.ascontiguousarray(np.asarray(inp["x"][b], dtype=np.float32))
    m["ctx"] = np.ascontiguousarray(np.asarray(inp["ctx"][b], dtype=np.float32))
    m["c_pj"] = np.ascontiguousarray(np.asarray(inp["c"][b], dtype=np.float32).reshape(8, 128).T)
    return m


def kernel(**inputs):
    sh = _shared_inputs(inputs)
    nb = inputs["x"].shape[0]
    nc = build_program()
    in_maps = [_core_inputs(inputs, sh, b) for b in range(nb)]
    res = run_bass_kernel_spmd(nc, in_maps, core_ids=list(range(nb)))
    return np.stack([np.asarray(r["out"], dtype=np.float32) for r in res.results], axis=0)
```
